# Optimizing a Trainium2 kernel written in Bass

```python
import jax, jax.numpy as jnp
from jax import lax
import numpy as np


D_MODEL = 2048
BATCH = 1
SEQ = 8192
DEPTH = 1

HEAD_DIM = 128
DIL_PATTERNS = ((128, 1), (512, 4), (2048, 16))
HEADS_PER_DIL_GROUP = 4
N_HEADS_A = HEADS_PER_DIL_GROUP * len(DIL_PATTERNS)
N_HEADS_B = 8
N_HEADS = N_HEADS_A + N_HEADS_B
WIDTH_A = N_HEADS_A * HEAD_DIM
WIDTH_A_OUT = HEADS_PER_DIL_GROUP * HEAD_DIM
WIDTH_B = N_HEADS_B * HEAD_DIM
QKV_COLS = 3 * (WIDTH_A + WIDTH_B)
BAND = 128
MOBA_BLOCK = 256
MOBA_TOPK = 3
MOBA_Q_CHUNK = 32
D_FF = 5632
N_ADA = 9
EPS = 1e-6
NEG_INF = -1e30

kernel_name = "hybrid_dilated_moba_macaron_block"


def rmsnorm(x, g):
    xf = x.astype(jnp.float32)
    y = xf * lax.rsqrt(jnp.mean(xf * xf, axis=-1, keepdims=True) + EPS)
    return (y * g.astype(jnp.float32)).astype(x.dtype)


def head_rmsnorm(a, g):
    af = a.astype(jnp.float32)
    y = af * lax.rsqrt(jnp.mean(af * af, axis=-1, keepdims=True) + EPS)
    return (y * g.astype(jnp.float32)[None, :, None, :]).astype(a.dtype)


def modulate(x, shift, scale):
    return x * (1.0 + scale[:, None, :]) + shift[:, None, :]


def swiglu(x, w_gate, w_up, w_down):
    return (jax.nn.silu(x @ w_gate) * (x @ w_up)) @ w_down


def alibi_slopes(n):
    return jnp.exp2(-8.0 * jnp.arange(1, n + 1, dtype=jnp.float32) / n)


def head_split(a, n_heads):
    b, t, _ = a.shape
    return a.reshape(b, t, n_heads, HEAD_DIM).transpose(0, 2, 1, 3)


def head_merge(a):
    b, h, t, d = a.shape
    return a.transpose(0, 2, 1, 3).reshape(b, t, h * d)


def dilated_window_attn(q, k, v, slopes, dilation):
    b, h, t, dh = q.shape
    r = dilation
    L = t // r
    n_blk = -(-L // BAND)
    Lp = n_blk * BAND

    def to_sub(a):
        return a.reshape(b, h, L, r, dh).transpose(0, 1, 3, 2, 4)

    pad_q = ((0, 0), (0, 0), (0, 0), (0, Lp - L), (0, 0))
    pad_kv = ((0, 0), (0, 0), (0, 0), (BAND, Lp - L), (0, 0))
    qb = jnp.pad(to_sub(q), pad_q).reshape(b, h, r, n_blk, BAND, dh)
    kb = jnp.pad(to_sub(k), pad_kv).reshape(b, h, r, n_blk + 1, BAND, dh)
    vb = jnp.pad(to_sub(v), pad_kv).reshape(b, h, r, n_blk + 1, BAND, dh)
    k_band = jnp.concatenate([kb[:, :, :, :-1], kb[:, :, :, 1:]], axis=4)
    v_band = jnp.concatenate([vb[:, :, :, :-1], vb[:, :, :, 1:]], axis=4)

    s = jnp.einsum('bhrnqd,bhrnkd->bhrnqk', qb, k_band).astype(jnp.float32) * (HEAD_DIM ** -0.5)
    qi = jnp.arange(BAND)[:, None]
    ki = jnp.arange(2 * BAND)[None, :]
    dist = BAND + qi - ki
    key_idx = jnp.arange(n_blk)[:, None, None] * BAND - BAND + ki[None]
    valid = (dist >= 0)[None] & (dist <= BAND)[None] & (key_idx >= 0)
    s = s - slopes[None, :, None, None, None, None] * (dist * r).astype(jnp.float32)
    s = jnp.where(valid, s, NEG_INF)
    m = jnp.max(s, axis=-1, keepdims=True)
    p = jnp.exp(s - m)
    denom = jnp.sum(p, axis=-1, keepdims=True)
    o = jnp.einsum('bhrnqk,bhrnkd->bhrnqd', (p / denom).astype(v.dtype), v_band)
    lse = (m + jnp.log(denom))[..., 0]
    o = o.reshape(b, h, r, Lp, dh)[:, :, :, :L].transpose(0, 1, 3, 2, 4).reshape(b, h, t, dh)
    lse = lse.reshape(b, h, r, Lp)[:, :, :, :L].transpose(0, 1, 3, 2).reshape(b, h, t)
    return o, lse


def moba_attn(q, k, v, slopes):
    b, h, t, dh = q.shape
    nb = -(-t // MOBA_BLOCK)
    tp = nb * MOBA_BLOCK
    pad = ((0, 0), (0, 0), (0, tp - t), (0, 0))
    kblk = jnp.pad(k, pad).reshape(b, h, nb, MOBA_BLOCK, dh)
    vblk = jnp.pad(v, pad).reshape(b, h, nb, MOBA_BLOCK, dh)
    kmean = jnp.mean(kblk.astype(jnp.float32), axis=3)
    gate = jnp.einsum('bhtd,bhnd->bhtn', q.astype(jnp.float32), kmean)
    pos = jnp.arange(t)
    qblk = pos // MOBA_BLOCK
    past = jnp.arange(nb)[None, :] < qblk[:, None]
    gate = jnp.where(past, gate, NEG_INF)
    k_sel = min(MOBA_TOPK, nb)
    _, top_idx = lax.top_k(gate, k_sel)
    top_valid = top_idx < qblk[:, None]
    own = jnp.broadcast_to(qblk[:, None], (b, h, t, 1)).astype(top_idx.dtype)
    sel_idx = jnp.concatenate([top_idx, own], axis=-1)
    sel_valid = jnp.concatenate([top_valid, jnp.ones((b, h, t, 1), dtype=bool)], axis=-1)

    nc = t // MOBA_Q_CHUNK

    def to_chunks(a):
        return jnp.moveaxis(a.reshape(b, h, nc, MOBA_Q_CHUNK, a.shape[-1]), 2, 0)

    q_c = to_chunks(q)
    idx_c = to_chunks(sel_idx)
    ok_c = to_chunks(sel_valid)
    pos_c = pos.reshape(nc, MOBA_Q_CHUNK)
    gather_blocks = jax.vmap(jax.vmap(lambda blk, i: blk[i]))

    def one_chunk(args):
        qx, ix, okx, px = args
        kg = gather_blocks(kblk, ix)
        vg = gather_blocks(vblk, ix)
        s = jnp.einsum('bhcd,bhcskd->bhcsk', qx, kg).astype(jnp.float32) * (HEAD_DIM ** -0.5)
        key_pos = ix[..., None] * MOBA_BLOCK + jnp.arange(MOBA_BLOCK)
        dist = px[None, None, :, None, None] - key_pos
        mask = okx[..., None] & (dist >= 0)
        s = s - slopes[None, :, None, None, None] * dist.astype(jnp.float32)
        s = jnp.where(mask, s, NEG_INF)
        p = jax.nn.softmax(s.reshape(b, h, MOBA_Q_CHUNK, -1), axis=-1).reshape(s.shape)
        return jnp.einsum('bhcsk,bhcskd->bhcd', p.astype(vg.dtype), vg)

    out = lax.map(one_chunk, (q_c, idx_c, ok_c, pos_c))
    return jnp.moveaxis(out, 0, 2).reshape(b, h, t, dh)


def hybrid_layer(x, c, w_ada, b_ada, g_ffn1, ffn1_w_gate, ffn1_w_up, ffn1_w_down,
                 g_mix, w_in, q_norm, k_norm, w_gate, w_branch_a, w_branch_b, w_out,
                 g_ffn2, ffn2_w_gate, ffn2_w_up, ffn2_w_down):
    ada = jax.nn.silu(c) @ w_ada + b_ada
    sh1, sc1, gt1, sh2, sc2, gt2, sh3, sc3, gt3 = jnp.split(ada, N_ADA, axis=-1)

    u = modulate(rmsnorm(x, g_ffn1), sh1, sc1)
    x = x + 0.5 * gt1[:, None, :] * swiglu(u, ffn1_w_gate, ffn1_w_up, ffn1_w_down)

    u = modulate(rmsnorm(x, g_mix), sh2, sc2)
    qkv = u @ w_in
    qa, ka, va, qb, kb, vb = jnp.split(
        qkv, np.cumsum([WIDTH_A, WIDTH_A, WIDTH_A, WIDTH_B, WIDTH_B])[:5].tolist(), axis=-1)
    qa = head_rmsnorm(head_split(qa, N_HEADS_A), q_norm[:N_HEADS_A])
    ka = head_rmsnorm(head_split(ka, N_HEADS_A), k_norm[:N_HEADS_A])
    va = head_split(va, N_HEADS_A)
    qb = head_rmsnorm(head_split(qb, N_HEADS_B), q_norm[N_HEADS_A:])
    kb = head_rmsnorm(head_split(kb, N_HEADS_B), k_norm[N_HEADS_A:])
    vb = head_split(vb, N_HEADS_B)
    slopes = alibi_slopes(N_HEADS)

    outs, lses = [], []
    for g, (_, dil) in enumerate(DIL_PATTERNS):
        hs = slice(g * HEADS_PER_DIL_GROUP, (g + 1) * HEADS_PER_DIL_GROUP)
        o, lse = dilated_window_attn(qa[:, hs], ka[:, hs], va[:, hs], slopes[hs], dil)
        outs.append(o)
        lses.append(lse)
    outs = jnp.stack(outs, axis=0)
    w_den = jax.nn.softmax(jnp.stack(lses, axis=0), axis=0)
    y_a = jnp.sum(w_den[..., None] * outs.astype(jnp.float32), axis=0).astype(x.dtype)
    y_a = head_merge(y_a)

    y_b = head_merge(moba_attn(qb, kb, vb, slopes[N_HEADS_A:]))

    g_a, g_b = jnp.split(jax.nn.sigmoid(u @ w_gate), 2, axis=-1)
    merged = g_a * (y_a @ w_branch_a) + g_b * (y_b @ w_branch_b)
    x = x + gt2[:, None, :] * (merged @ w_out)

    u = modulate(rmsnorm(x, g_ffn2), sh3, sc3)
    x = x + 0.5 * gt3[:, None, :] * swiglu(u, ffn2_w_gate, ffn2_w_up, ffn2_w_down)
    return x


def setup_inputs(seed: int = 0) -> dict:
    key = jax.random.key(seed)
    ks = jax.random.split(key, 24)

    def nrm(k, shape, scale):
        return jax.random.normal(k, shape, jnp.float32) * scale

    def gain(k, shape):
        return 1.0 + 0.01 * jax.random.normal(k, shape, jnp.float32)

    d = D_MODEL
    return {
        'x': nrm(ks[0], (BATCH, SEQ, d), 1.0),
        'c': nrm(ks[1], (BATCH, d), 1.0),
        'w_ada': nrm(ks[2], (DEPTH, d, N_ADA * d), d ** -0.5),
        'b_ada': nrm(ks[3], (DEPTH, N_ADA * d), 0.01),
        'g_ffn1': gain(ks[4], (DEPTH, d)),
        'ffn1_w_gate': nrm(ks[5], (DEPTH, d, D_FF), d ** -0.5),
        'ffn1_w_up': nrm(ks[6], (DEPTH, d, D_FF), d ** -0.5),
        'ffn1_w_down': nrm(ks[7], (DEPTH, D_FF, d), D_FF ** -0.5),
        'g_mix': gain(ks[8], (DEPTH, d)),
        'w_in': nrm(ks[9], (DEPTH, d, QKV_COLS), d ** -0.5),
        'q_norm': gain(ks[10], (DEPTH, N_HEADS, HEAD_DIM)),
        'k_norm': gain(ks[11], (DEPTH, N_HEADS, HEAD_DIM)),
        'w_gate': nrm(ks[12], (DEPTH, d, 2 * d), d ** -0.5),
        'w_branch_a': nrm(ks[13], (DEPTH, WIDTH_A_OUT, d), WIDTH_A_OUT ** -0.5),
        'w_branch_b': nrm(ks[14], (DEPTH, WIDTH_B, d), WIDTH_B ** -0.5),
        'w_out': nrm(ks[15], (DEPTH, d, d), d ** -0.5),
        'g_ffn2': gain(ks[16], (DEPTH, d)),
        'ffn2_w_gate': nrm(ks[17], (DEPTH, d, D_FF), d ** -0.5),
        'ffn2_w_up': nrm(ks[18], (DEPTH, d, D_FF), d ** -0.5),
        'ffn2_w_down': nrm(ks[19], (DEPTH, D_FF, d), D_FF ** -0.5),
    }


def reference(x, c, w_ada, b_ada, g_ffn1, ffn1_w_gate, ffn1_w_up, ffn1_w_down,
              g_mix, w_in, q_norm, k_norm, w_gate, w_branch_a, w_branch_b, w_out,
              g_ffn2, ffn2_w_gate, ffn2_w_up, ffn2_w_down):
    for l in range(DEPTH):
        x = hybrid_layer(x, c, w_ada[l], b_ada[l], g_ffn1[l], ffn1_w_gate[l], ffn1_w_up[l],
                         ffn1_w_down[l], g_mix[l], w_in[l], q_norm[l], k_norm[l], w_gate[l],
                         w_branch_a[l], w_branch_b[l], w_out[l], g_ffn2[l], ffn2_w_gate[l],
                         ffn2_w_up[l], ffn2_w_down[l])
    return x
```

```python
import numpy as np
import ml_dtypes
import concourse.bass as bass
import concourse.mybir as mybir
from concourse.bass_utils import run_bass_kernel_spmd

F32 = mybir.dt.float32
BF = mybir.dt.bfloat16
AF = mybir.ActivationFunctionType
ALU = mybir.AluOpType
AX = mybir.AxisListType

D = 2048
T = 8192
NC = 8
TL = 1024
DFF = 5632
NF = 44
EPS = 1e-6
NEG = -30000.0
DILS = (1, 4, 16)


def _slopes():
    return np.exp2(-8.0 * np.arange(1, 21, dtype=np.float32) / 20).astype(np.float32)


class _Ctr:
    def __init__(self, sem):
        self.sem = sem
        self.n = 0


class _B:
    def __init__(self, nc):
        self.nc = nc
        self.q = {k: [] for k in ("pe", "act", "dve", "pool", "sp")}
        self.ctr = {}
        self.waited = {}
        self.free = {}
        self.nsem = 0

    def newsem(self, name):
        self.nsem += 1
        return _Ctr(self.nc.semaphore(name).__enter__())

    def op(self, eng, fn, sig=False):
        if sig:
            c = self.ctr[eng]
            c.n += 1
            tgt = c.n
            self.q[eng].append(lambda e, fn=fn, c=c: fn(e).then_inc(c.sem, 1))
            return (c, tgt)
        self.q[eng].append(fn)
        return None

    def wait(self, eng, ev):
        if ev is None:
            return
        c, tgt = ev
        key = (eng, id(c))
        if self.waited.get(key, 0) >= tgt:
            return
        self.waited[key] = tgt
        self.q[eng].append(lambda e, c=c, tgt=tgt: e.wait_ge(c.sem, tgt))

    def dma(self, eng, out, in_, dsem):
        dsem.n += 16
        self.q[eng].append(lambda e, o=out, i=in_, d=dsem: e.dma_start(out=o, in_=i).then_inc(d.sem, 16))
        return (dsem, dsem.n)

    def last(self, eng):
        c = self.ctr[eng]
        return (c, c.n) if c.n > 0 else None


def _build(debug=False):
    nc = bass.Bass("TRN2", target_bir_lowering=False)
    B = _B(nc)

    def din(name, shape, dt=F32):
        return nc.dram_tensor(name, list(shape), dt, kind="ExternalInput").ap()

    xT_d = din("xT", [8, 128, 16 * TL])
    csb_d = din("csb", [128, 16])
    wada_d = din("wada", [48, 128, 16 * 384])
    bada_d = din("bada", [128, 144])
    gains_d = din("gains", [128, 48])
    qkg_d = din("qkg", [128, 40])
    f_wgu = [din("f1_wgu", [NF, 128, 4096]), din("f2_wgu", [NF, 128, 4096])]
    f_wd = [din("f1_wd", [NF, 128, 2048]), din("f2_wd", [NF, 128, 2048])]
    wk_d = din("wk", [20, 128, 2048])
    wq_d = din("wq", [20, 128, 2048])
    wv_d = din("wv", [10, 128, 16 * 256])
    wmg_d = din("wmg", [16, 128, 5632])
    wo_d = din("wo", [16, 128, 2048])
    pm_d = din("pm", [128, 256])
    past_d = din("past01", [128, 256])
    own_d = din("own01", [128, 256])
    mb_d = din("mbias", [128, 8 * 128])
    db_d = din("dbias", [128, 512])
    esel_d = din("esel", [32, 32 * 128], BF)
    ownm_d = din("ownmask", [128, 4 * 512], BF)
    wtab_d = din("wtab", [128, 5376], BF)
    sl6_d = din("sl6", [6, 12 * 128], BF)
    q6_d = din("q6", [6, 512], BF)
    ident_d = din("ident", [128, 128], BF)
    out_d = nc.dram_tensor("outT", [128, 16 * TL], F32, kind="ExternalOutput").ap()
    kd = "ExternalOutput" if debug else "Internal"
    kT_all = nc.dram_tensor("kT_all", [20 * 128, T], BF, kind=kd).ap()
    qT_all = nc.dram_tensor("qT_all", [20 * 128, TL], BF, kind=kd).ap()
    v_all = nc.dram_tensor("v_all", [T, 2560], BF, kind=kd).ap()
    mT_all = nc.dram_tensor("mT_all", [8 * 32, TL], BF, kind=kd).ap()
    if debug:
        dbg_x1 = nc.dram_tensor("dbg_x1", [128, 16 * TL], F32, kind="ExternalOutput").ap()
        dbg_x2 = nc.dram_tensor("dbg_x2", [128, 16 * TL], F32, kind="ExternalOutput").ap()
        dbg_u2 = nc.dram_tensor("dbg_u2", [128, 16 * TL], BF, kind="ExternalOutput").ap()
        dbg_y = nc.dram_tensor("dbg_y", [128, 12 * TL], BF, kind="ExternalOutput").ap()
        dbg_ada = nc.dram_tensor("dbg_ada", [128, 144], F32, kind="ExternalOutput").ap()

    BASE = (int(nc.sbuf_base) + 63) // 64 * 64
    TOP = int(nc.sbuf_top) // 64 * 64
    off = [BASE]

    def sb(name, shape, dt, at=None):
        nb = int(np.prod(shape[1:])) * (4 if dt == F32 else 2)
        if at is None:
            at = off[0]
            off[0] += (nb + 63) // 64 * 64
        return nc.alloc_sbuf_tensor_at(name, list(shape), dt, offset=at), at + nb

    xT, _ = sb("xTs", [128, 16, TL], F32)
    uT, _ = sb("uTs", [128, 16, TL], BF)
    ada, _ = sb("ada", [128, 144], F32)
    cols, _ = sb("cols", [128, 80], F32)
    gains, _ = sb("gains_s", [128, 48], F32)
    qkg, _ = sb("qkg_s", [128, 40], F32)
    csb, _ = sb("csb_s", [128, 16], F32)
    cbf, _ = sb("cbf", [128, 16], BF)
    ones, _ = sb("ones", [128, 128], BF)
    ident, _ = sb("ident_s", [128, 128], BF)
    ksum, _ = sb("ksum", [128, 8, 32], F32)
    ksb, _ = sb("ksb", [128, 8, 32], BF)
    ksl, _ = sb("ksl", [128, 8, 32], BF)
    ksr, _ = sb("ksr", [128, 8, 32], F32)
    zcol, _ = sb("zcol", [128, 1], F32)
    epsc, _ = sb("epsc", [128, 2], F32)
    bada, _ = sb("bada_s", [128, 144], F32)
    C0 = off[0]
    off[0] = C0
    ring = [sb(f"ring{i}", [128, 6144], BF)[0] for i in range(4)]
    sg = [sb(f"sg{i}", [128, 512], F32)[0] for i in range(2)]
    tmp = [sb(f"tmp{i}", [128, 512], F32)[0] for i in range(2)]
    P4_end = off[0]
    hT, _ = sb("hT", [128, 2, TL], BF)
    sq = [sb(f"sq{i}", [128, 512], BF)[0] for i in range(2)]
    rstd, _ = sb("rstd", [128, TL], F32)
    kst = [sb(f"kst{i}", [128, 512], BF)[0] for i in range(2)]
    vst = [sb(f"vst{i}", [128, 256], BF)[0] for i in range(2)]
    Cd_end = off[0]
    merged, _ = sb("merged", [128, 16, 512], BF, at=TOP - 16384 - 24576)
    yT_at = TOP - 24576
    yT, _ = sb("yT", [128, 12, TL], BF, at=yT_at)
    off[0] = C0
    kbuf = [sb(f"kbuf{i}", [128, 4096], BF)[0] for i in range(2)]
    vbuf = [sb(f"vbuf{i}", [128, 32, 128], BF)[0] for i in range(2)]
    esel, _ = sb("esel_s", [32, 32 * 128], BF)
    ownm, _ = sb("ownm_s", [128, 4 * 512], BF)
    wtab, _ = sb("wtab_s", [128, 5376], BF)
    sl6, _ = sb("sl6_s", [6, 12 * 128], BF)
    q6, _ = sb("q6_s", [6, 512], BF)
    Pb = [sb(f"P{i}", [128, 512], BF)[0] for i in range(3)]
    QTh, _ = sb("QTh", [128, TL], BF)
    MTh, _ = sb("MTh", [32, TL], BF)
    pm, _ = sb("pm_s", [128, 256], F32)
    past, _ = sb("past_s", [128, 256], F32)
    own01, _ = sb("own_s", [128, 256], F32)
    mb, _ = sb("mb_s", [128, 8 * 128], F32)
    db, _ = sb("db_s", [128, 512], F32)
    gm, _ = sb("gm", [128, 256], F32)
    selt, _ = sb("selt", [128, 256], F32)
    madd, _ = sb("madd", [128, 256], BF)
    mx8, _ = sb("mx8", [128, 8], F32)
    mst, _ = sb("mst", [32, TL], BF)
    rden, _ = sb("rden", [128, 512], F32)
    assert off[0] <= yT_at, (off[0], yT_at)
    assert P4_end <= yT_at - 16384, (P4_end, yT_at)
    assert Cd_end <= TOP, Cd_end

    ps = [nc.alloc_psum_tensor(f"ps{i}", [128, 512], F32) for i in range(7)]
    psT = nc.alloc_psum_tensor("psT", [128, 1024], BF)

    for e in ("pe", "act", "dve", "pool", "sp"):
        B.ctr[e] = B.newsem("c_" + e)
    s_misc = B.newsem("d_misc")
    s_x = B.newsem("d_x")
    s_st = B.newsem("d_st")
    s_full = [B.newsem(f"d_ring{i}") for i in range(4)]
    s_kv = [B.newsem(f"d_kv{i}") for i in range(2)]
    s_qm = B.newsem("d_qm")
    s_tab = B.newsem("d_tab")
    s_out = B.newsem("d_out")

    def mm(bank_key, out_ap, pairs, first=True, last=True, extra_wait=()):
        B.wait("pe", B.free.get(bank_key))
        for w in extra_wait:
            B.wait("pe", w)
        ev = None
        n = len(pairs)
        for i, (l, r) in enumerate(pairs):
            st = first and i == 0
            sp = last and i == n - 1
            ev = B.op("pe", lambda e, o=out_ap, l=l, r=r, st=st, sp=sp: e.matmul(o, lhsT=l, rhs=r, start=st, stop=sp),
                      sig=(i == n - 1))
        return ev

    ring_state = {"n": 0, "rel": {}}

    def wload(dmas):
        n = ring_state["n"]
        ring_state["n"] += 1
        slot = n % 4
        B.wait("pool", ring_state["rel"].get(n - 4))
        ev = None
        for (o_fn, i_ap) in dmas:
            ev = B.dma("pool", o_fn(ring[slot]), i_ap, s_full[slot])
        return {"n": n, "slot": slot, "ev": ev, "t": ring[slot]}

    def wrelease(h, ev):
        ring_state["rel"][h["n"]] = ev

    def run_stages(stages):
        handles = {}
        nxt = 0
        for i, (dmas, comp) in enumerate(stages):
            while nxt < len(stages) and nxt <= i + 2:
                handles[nxt] = wload(stages[nxt][0])
                nxt += 1
            h = handles.pop(i)
            B.wait("pe", h["ev"])
            comp(h)
            wrelease(h, B.last("pe"))

    def barrier():
        evs = [B.last(e) for e in ("pe", "act", "dve")]
        for e in ("pe", "act", "dve", "pool", "sp"):
            for ev in evs:
                B.wait(e, ev)

    evm = None
    for (dst, src) in ((csb, csb_d), (bada, bada_d), (gains, gains_d), (qkg, qkg_d), (ident, ident_d)):
        evm = B.dma("sp", dst[:], src, s_misc)
    B.wait("act", evm)
    B.wait("dve", evm)
    B.op("dve", lambda e: e.memset(ones[:], 1.0))
    B.op("dve", lambda e: e.memset(zcol[:], 0.0))
    B.op("dve", lambda e: e.memset(epsc[:, 0:1], float(D * EPS)))
    B.op("dve", lambda e: e.memset(epsc[:, 1:2], float(128 * EPS)))
    B.op("dve", lambda e: e.memset(ksum[:], 0.0))
    ev_g = B.op("dve", lambda e: e.tensor_scalar(out=gains[:], in0=gains[:], scalar1=float(np.sqrt(D)), scalar2=None,
                                                 op0=ALU.mult), sig=True)
    ev_kg = B.op("dve", lambda e: e.tensor_scalar(out=qkg[:, 20:40], in0=qkg[:, 20:40], scalar1=float(np.sqrt(128.0)),
                                                  scalar2=None, op0=ALU.mult), sig=True)
    ev_c = B.op("act", lambda e: e.activation(out=cbf[:], in_=csb[:], func=AF.Silu), sig=True)

    ada_ps = ps[6]
    st_list = []
    for blk in range(48):
        def comp(h, blk=blk):
            t = h["t"]
            for jl in range(3):
                j = 3 * blk + jl
                pairs = [(t[:, kc * 384 + jl * 128: kc * 384 + jl * 128 + 128], cbf[:, kc:kc + 1]) for kc in range(16)]
                mm("ada", ada_ps[:, j:j + 1], pairs, extra_wait=(ev_c, ev_g))
        st_list.append(([(lambda t: t[:, :], wada_d[blk])], comp))
    run_stages(st_list)
    B.wait("dve", B.last("pe"))
    ev_ada = B.op("dve", lambda e: e.tensor_tensor(out=ada[:], in0=ada_ps[:, 0:144], in1=bada[:], op=ALU.add), sig=True)
    B.wait("dve", ev_ada)
    for i, (sc_i, g_i) in enumerate(((1, 0), (4, 1), (7, 2))):
        B.op("dve", lambda e, i=i, sc_i=sc_i, g_i=g_i: e.scalar_tensor_tensor(
            out=cols[:, 16 * i:16 * i + 16], in0=ada[:, 16 * sc_i:16 * sc_i + 16], scalar=1.0,
            in1=gains[:, 16 * g_i:16 * g_i + 16], op0=ALU.add, op1=ALU.mult))
    B.op("dve", lambda e: e.tensor_scalar(out=cols[:, 48:64], in0=ada[:, 32:48], scalar1=0.5, scalar2=None, op0=ALU.mult))
    ev_cols = B.op("dve", lambda e: e.tensor_scalar(out=cols[:, 64:80], in0=ada[:, 128:144], scalar1=0.5, scalar2=None,
                                                    op0=ALU.mult), sig=True)
    B.wait("act", ev_cols)
    B.wait("dve", ev_cols)
    B.free["ada"] = ev_ada

    cnt = {"sq": 0, "tmp": 0, "sg": 0, "kst": 0, "vst": 0, "pa": 0}

    def norm_mod(a_off, sh_off, ev_x):
        for tt in range(2):
            tsl = slice(tt * 512, tt * 512 + 512)
            for k in range(16):
                i = cnt["sq"]; cnt["sq"] += 1
                B.wait("act", B.free.get(("sq", i % 2)))
                B.wait("act", ev_x)
                evs = B.op("act", lambda e, i=i, k=k, tsl=tsl: e.activation(out=sq[i % 2][:], in_=xT[:, k, tsl], func=AF.Square), sig=True)
                evp = mm("S", ps[6][:, :], [(ones[:, :], sq[i % 2][:])], first=(k == 0), last=(k == 15), extra_wait=(evs,))
                B.free[("sq", i % 2)] = evp
            B.wait("act", evp)
            evr0 = B.op("act", lambda e, tsl=tsl: e.activation(out=rstd[:, tsl], in_=ps[6][:, :], func=AF.Sqrt, bias=epsc[:, 0:1], scale=1.0), sig=True)
            B.free["S"] = evr0
            B.wait("dve", evr0)
            evr = B.op("dve", lambda e, tsl=tsl: e.reciprocal(out=rstd[:, tsl], in_=rstd[:, tsl]), sig=True)
            B.wait("dve", evr)
            for k in range(16):
                i = cnt["tmp"]; cnt["tmp"] += 1
                B.wait("dve", B.free.get(("tmp", i % 2)))
                B.wait("dve", ev_x)
                evt = B.op("dve", lambda e, i=i, k=k, tsl=tsl: e.scalar_tensor_tensor(
                    out=tmp[i % 2][:], in0=xT[:, k, tsl], scalar=cols[:, a_off + k:a_off + k + 1], in1=rstd[:, tsl],
                    op0=ALU.mult, op1=ALU.mult), sig=True)
                B.wait("act", evt)
                eva = B.op("act", lambda e, i=i, k=k, tsl=tsl: e.activation(out=uT[:, k, tsl], in_=tmp[i % 2][:], func=AF.Identity,
                                                                   bias=ada[:, sh_off + k:sh_off + k + 1], scale=1.0), sig=True)
                B.free[("tmp", i % 2)] = eva
        return B.last("act")

    def ffn(fi, hg_off, ev_u):
        stages = []
        for f in range(NF):
            def comp(h, f=f):
                t = h["t"]
                for tt in range(2):
                    tsl = slice(tt * 512, tt * 512 + 512)
                    par = (f * 2 + tt) % 2
                    evg = mm(("A", par), ps[par][:, :], [(t[:, kc * 128:(kc + 1) * 128], uT[:, kc, tsl]) for kc in range(16)],
                             extra_wait=(ev_u,))
                    evu = mm(("Bk", par), ps[2 + par][:, :],
                             [(t[:, 2048 + kc * 128:2048 + (kc + 1) * 128], uT[:, kc, tsl]) for kc in range(16)])
                    i = cnt["sg"]; cnt["sg"] += 1
                    B.wait("act", evg)
                    B.wait("act", B.free.get(("sg", i % 2)))
                    evs = B.op("act", lambda e, i=i, par=par: e.activation(out=sg[i % 2][:], in_=ps[par][:, :], func=AF.Silu), sig=True)
                    B.free[("A", par)] = evs
                    B.wait("dve", evs)
                    B.wait("dve", evu)
                    B.wait("dve", B.free.get("hT"))
                    evh = B.op("dve", lambda e, i=i, par=par, f=f, tsl=tsl: e.tensor_tensor(
                        out=hT[:, f % 2, tsl], in0=sg[i % 2][:], in1=ps[2 + par][:, :], op=ALU.mult), sig=True)
                    B.free[("sg", i % 2)] = evh
                    B.free[("Bk", par)] = evh
                ffn_state["h"].append(h)
                if f % 2 == 1:
                    hs = ffn_state["h"]
                    ffn_state["h"] = []
                    evh_all = B.last("dve")
                    for n in range(16):
                        for tt in range(2):
                            tsl = slice(tt * 512, tt * 512 + 512)
                            par = (n * 2 + tt) % 2
                            evd = mm(("Cc", par), ps[4 + par][:, :],
                                     [(hh["t"][:, 4096 + n * 128:4096 + (n + 1) * 128], hT[:, j, tsl]) for j, hh in enumerate(hs)],
                                     extra_wait=(evh_all,))
                            B.wait("dve", evd)
                            evx = B.op("dve", lambda e, n=n, par=par, tsl=tsl: e.scalar_tensor_tensor(
                                out=xT[:, n, tsl], in0=ps[4 + par][:, :], scalar=cols[:, hg_off + n:hg_off + n + 1],
                                in1=xT[:, n, tsl], op0=ALU.mult, op1=ALU.add), sig=True)
                            B.free[("Cc", par)] = evx
                    B.free["hT"] = B.last("pe")
                    for hh in hs[:-1]:
                        wrelease(hh, B.last("pe"))
            stages.append(([(lambda t: t[:, 0:4096], f_wgu[fi][f]), (lambda t: t[:, 4096:6144], f_wd[fi][f])], comp))
        return stages

    ffn_state = {"h": []}

    def headproj(w_d, gcol_off, dst_all, col0, nheads, ev_u, do_ksum_blk=None):
        stages = []
        groups = [list(range(g, min(g + 3, nheads))) for g in range(0, nheads, 3)]
        for grp in groups:
            def comp(h, grp=grp):
                t = h["t"]
                for hi, hd in enumerate(grp):
                    for tt in range(2):
                        tsl = slice(tt * 512, tt * 512 + 512)
                        par = cnt["pa"] % 2; cnt["pa"] += 1
                        evq = mm(("A", par), ps[par][:, :],
                                 [(t[:, hi * 2048 + kc * 128: hi * 2048 + (kc + 1) * 128], uT[:, kc, tsl]) for kc in range(16)],
                                 extra_wait=(ev_u,))
                        i = cnt["sq"]; cnt["sq"] += 1
                        B.wait("act", evq)
                        B.wait("act", B.free.get(("sq", i % 2)))
                        evs = B.op("act", lambda e, i=i, par=par: e.activation(out=sq[i % 2][:], in_=ps[par][:, :], func=AF.Square), sig=True)
                        evp = mm(("Bk", par), ps[2 + par][:, :], [(ones[:, :], sq[i % 2][:])], extra_wait=(evs,))
                        B.free[("sq", i % 2)] = evp
                        j = cnt["tmp"]; cnt["tmp"] += 1
                        B.wait("act", evp)
                        B.wait("act", B.free.get(("tmp", j % 2)))
                        evr0 = B.op("act", lambda e, j=j, par=par: e.activation(out=tmp[j % 2][:], in_=ps[2 + par][:, :], func=AF.Sqrt,
                                                                                 bias=epsc[:, 1:2], scale=1.0), sig=True)
                        B.free[("Bk", par)] = evr0
                        B.wait("dve", evr0)
                        evr = B.op("dve", lambda e, j=j: e.reciprocal(out=tmp[j % 2][:], in_=tmp[j % 2][:]), sig=True)
                        B.wait("dve", evr)
                        kk = cnt["kst"]; cnt["kst"] += 1
                        B.wait("dve", (s_st, s_st.n))
                        evk = B.op("dve", lambda e, j=j, kk=kk, par=par, hd=hd: e.scalar_tensor_tensor(
                            out=kst[kk % 2][:], in0=ps[par][:, :], scalar=qkg[:, gcol_off + hd:gcol_off + hd + 1],
                            in1=tmp[j % 2][:], op0=ALU.mult, op1=ALU.mult), sig=True)
                        B.free[("A", par)] = evk
                        B.free[("tmp", j % 2)] = evk
                        if do_ksum_blk is not None and hd >= 12:
                            B.wait("dve", evk)
                            b4 = 4 * do_ksum_blk + 2 * tt
                            B.op("dve", lambda e, kk=kk, hd=hd, b4=b4: e.tensor_reduce(
                                out=ksum[:, hd - 12, b4:b4 + 2], in_=kst[kk % 2][:].rearrange("p (a b) -> p a b", b=256),
                                axis=AX.X, op=ALU.add), sig=True)
                        B.wait("sp", B.last("dve"))
                        evst = B.dma("sp", dst_all[hd * 128:(hd + 1) * 128, col0 + tt * 512: col0 + tt * 512 + 512],
                                     kst[kk % 2][:], s_st)
                        B.free[("kst", kk % 2)] = evst
            dmas = [(lambda t, hi=hi: t[:, hi * 2048:(hi + 1) * 2048], w_d[hd]) for hi, hd in enumerate(grp)]
            stages.append((dmas, comp))
        return stages

    def vproj(b, ev_u):
        stages = []
        for ct in range(10):
            def comp(h, ct=ct):
                t = h["t"]
                for tk in range(8):
                    par = cnt["pa"] % 2; cnt["pa"] += 1
                    evv = mm(("Cc", par), ps[4 + par][:, 0:256],
                             [(uT[:, kc, tk * 128:(tk + 1) * 128], t[:, kc * 256:(kc + 1) * 256]) for kc in range(16)],
                             extra_wait=(ev_u,))
                    i = cnt["vst"]; cnt["vst"] += 1
                    B.wait("act", evv)
                    B.wait("act", (s_st, s_st.n))
                    evc = B.op("act", lambda e, i=i, par=par: e.activation(out=vst[i % 2][:], in_=ps[4 + par][:, 0:256], func=AF.Copy), sig=True)
                    B.free[("Cc", par)] = evc
                    B.wait("sp", evc)
                    evst = B.dma("sp", v_all[b * 1024 + tk * 128: b * 1024 + (tk + 1) * 128, ct * 256:(ct + 1) * 256],
                                 vst[i % 2][:], s_st)
                    B.free[("vst", i % 2)] = evst
            stages.append(([(lambda t: t[:, 0:4096], wv_d[ct])], comp))
        return stages

    ev_xdone = None
    for b in range(8):
        for e in ("act", "dve", "pe"):
            B.wait("sp", B.last(e))
        evx = B.dma("sp", xT[:].rearrange("p k t -> p (k t)"), xT_d[b], s_x)
        B.wait("act", evx)
        B.wait("dve", evx)
        ev_u = norm_mod(0, 0, evx)
        run_stages(ffn(0, 48, ev_u))
        ev_x1 = B.last("dve")
        B.wait("act", ev_x1)
        ev_u2 = norm_mod(16, 48, ev_x1)
        if debug and b == 7:
            B.wait("sp", ev_u2); B.wait("sp", ev_x1)
            B.dma("sp", dbg_x1, xT[:].rearrange("p k t -> p (k t)"), s_st)
            B.dma("sp", dbg_u2, uT[:].rearrange("p k t -> p (k t)"), s_st)
            B.dma("sp", dbg_ada, ada[:], s_st)
        st = headproj(wk_d, 20, kT_all, b * 1024, 20, ev_u2, do_ksum_blk=b)
        if b == 7:
            st += headproj(wq_d, 0, qT_all, 0, 20, ev_u2)
        st += vproj(b, ev_u2)
        run_stages(st)

    barrier()
    B.wait("sp", (s_st, s_st.n))
    evt = None
    for (dst, src) in ((esel, esel_d), (ownm, ownm_d), (wtab, wtab_d), (sl6, sl6_d), (q6, q6_d), (pm, pm_d),
                       (past, past_d), (own01, own_d), (mb, mb_d), (db, db_d)):
        evt = B.dma("sp", dst[:], src, s_tab)
    for e in ("pe", "act", "dve"):
        B.wait(e, evt)
    ev_k1 = B.op("dve", lambda e: e.tensor_copy(out=ksb[:], in_=ksum[:]), sig=True)
    B.wait("dve", ev_k1)
    ev_k2 = B.op("dve", lambda e: e.tensor_tensor(out=ksr[:], in0=ksum[:], in1=ksb[:], op=ALU.subtract), sig=True)
    B.wait("dve", ev_k2)
    ev_ksb = B.op("dve", lambda e: e.tensor_copy(out=ksl[:], in_=ksr[:]), sig=True)

    for j in range(8):
        B.wait("sp", B.free.get("QTh"))
        evq = B.dma("sp", QTh[:], qT_all[(12 + j) * 128:(13 + j) * 128, :], s_qm)
        evg = None
        B.wait("pe", B.free.get("G"))
        for stq in range(8):
            evg = mm("Gx", ps[6][:, stq * 32:(stq + 1) * 32],
                     [(QTh[:, stq * 128:(stq + 1) * 128], ksb[:, j, :]), (QTh[:, stq * 128:(stq + 1) * 128], ksl[:, j, :])],
                     extra_wait=(evq, ev_ksb))
        B.free["QTh"] = evg
        B.wait("dve", evg)
        ev1 = B.op("dve", lambda e: e.tensor_tensor(out=gm[:], in0=ps[6][:, 0:256], in1=pm[:], op=ALU.add), sig=True)
        B.free["G"] = ev1
        B.wait("dve", ev1)
        for stq in range(8):
            ev2 = B.op("dve", lambda e, stq=stq: e.max(out=mx8[:], in_=gm[:, stq * 32:(stq + 1) * 32]), sig=True)
            B.wait("dve", ev2)
            ev3 = B.op("dve", lambda e, stq=stq: e.tensor_scalar(out=selt[:, stq * 32:(stq + 1) * 32], in0=gm[:, stq * 32:(stq + 1) * 32],
                                                                  scalar1=mx8[:, 2:3], scalar2=None, op0=ALU.is_ge), sig=True)
            B.wait("dve", ev3)
        ev4a = B.op("dve", lambda e: e.tensor_tensor(out=selt[:], in0=selt[:], in1=past[:], op=ALU.mult), sig=True)
        B.wait("dve", ev4a)
        ev4 = B.op("dve", lambda e: e.tensor_tensor(out=selt[:], in0=selt[:], in1=own01[:], op=ALU.add), sig=True)
        B.wait("dve", ev4)
        B.wait("dve", B.free.get("madd"))
        ev5 = B.op("dve", lambda e: e.tensor_scalar(out=madd[:], in0=selt[:], scalar1=-1.0, scalar2=-NEG, op0=ALU.add, op1=ALU.mult), sig=True)
        B.wait("pe", ev5)
        B.wait("pe", B.free.get("psT"))
        evtr = None
        for stq in range(8):
            evtr = B.op("pe", lambda e, stq=stq: e.transpose(out=psT[0:32, stq * 128:(stq + 1) * 128], in_=madd[:, stq * 32:(stq + 1) * 32],
                                                             identity=ident[:, :]), sig=True)
        B.free["madd"] = evtr
        B.wait("act", evtr)
        B.wait("act", (s_st, s_st.n))
        evc = B.op("act", lambda e: e.activation(out=mst[:], in_=psT[0:32, :], func=AF.Copy), sig=True)
        B.free["psT"] = evc
        B.wait("sp", evc)
        evm2 = B.dma("sp", mT_all[j * 32:(j + 1) * 32, :], mst[:], s_st)
        B.free["mst"] = evm2
    B.wait("sp", (s_st, s_st.n))

    att = {"i": 0, "pend": None, "kv": 0}

    def flush():
        if att["pend"] is not None:
            att["pend"]()
            att["pend"] = None

    def pair(k_ap, q_ap, extras, bias_ap, v_ap, qt, first, last, ev_in):
        i = att["i"]; att["i"] += 1
        sbk = i % 2
        prs = [(k_ap, q_ap)] + extras
        evS = mm(("S2", sbk), ps[sbk][:, :], prs, extra_wait=ev_in)
        flush()
        B.wait("act", evS)
        B.wait("act", B.free.get(("P", i % 3)))
        evP = B.op("act", lambda e, i=i, sbk=sbk: e.activation(out=Pb[i % 3][:], in_=ps[sbk][:, :], func=AF.Exp, bias=bias_ap, scale=1.0), sig=True)
        B.free[("S2", sbk)] = evP

        def pv(i=i, evP=evP):
            B.wait("pe", evP)
            mm(("num", qt), ps[2 + qt][:, :], [(v_ap, Pb[i % 3][:])], first=first, last=last)
            ev = mm(("den", qt), ps[4 + qt][:, :], [(ones[:, :], Pb[i % 3][:])], first=first, last=last)
            B.free[("P", i % 3)] = ev
        att["pend"] = pv

    def finish(y_idx):
        flush()
        evl = B.last("pe")
        B.wait("dve", evl)
        for qt in range(2):
            ev1 = B.op("dve", lambda e, qt=qt: e.reciprocal(out=rden[:], in_=ps[4 + qt][:, :]), sig=True)
            B.wait("dve", ev1)
            ev2 = B.op("dve", lambda e, qt=qt: e.tensor_tensor(out=yT[:, y_idx, qt * 512:(qt + 1) * 512], in0=ps[2 + qt][:, :], in1=rden[:], op=ALU.mult), sig=True)
            B.wait("dve", ev2)
            B.free[("num", qt)] = ev2
            B.free[("den", qt)] = ev2

    def load_kv(h, tok0, ntok):
        i = att["kv"]; att["kv"] += 1
        bi = i % 2
        B.wait("sp", B.free.get(("kv", bi)))
        B.dma("sp", kbuf[bi][:, 0:ntok], kT_all[h * 128:(h + 1) * 128, tok0:tok0 + ntok], s_kv[bi])
        ev = None
        for a0 in range(0, ntok // 128, 8):
            ev = B.dma("sp", vbuf[bi][:, a0:a0 + 8, :],
                       v_all[tok0 + a0 * 128:tok0 + (a0 + 8) * 128, h * 128:(h + 1) * 128].rearrange("(a p) d -> p a d", p=128), s_kv[bi])
        return bi, ev

    def load_q(h, mj=None):
        B.wait("sp", B.free.get("QTh"))
        ev = B.dma("sp", QTh[:], qT_all[h * 128:(h + 1) * 128, :], s_qm)
        if mj is not None:
            ev = B.dma("sp", MTh[:], mT_all[mj * 32:(mj + 1) * 32, :], s_qm)
        return ev

    for j in range(8):
        h = 12 + j
        evq = load_q(h, j)
        first = [True, True]
        for half in range(2):
            bi, evkv = load_kv(h, half * 4096, 4096)
            for ktl in range(32):
                kt = half * 32 + ktl
                for qt in range(2):
                    if kt > 59 + 4 * qt:
                        continue
                    extras = [(esel[:, (kt // 2) * 128:(kt // 2 + 1) * 128], MTh[:, qt * 512:(qt + 1) * 512])]
                    jj = kt - 56 - 4 * qt
                    if 0 <= jj <= 3:
                        extras.append((ident[:, :], ownm[:, jj * 512:(jj + 1) * 512]))
                    last = (kt == 59 + 4 * qt)
                    pair(kbuf[bi][:, ktl * 128:(ktl + 1) * 128], QTh[:, qt * 512:(qt + 1) * 512], extras,
                         mb[:, j * 128 + kt * 2 + qt: j * 128 + kt * 2 + qt + 1], vbuf[bi][:, ktl, :], qt, first[qt], last, (evq, evkv))
                    first[qt] = False
            flush()
            B.free[("kv", bi)] = B.last("pe")
        B.free["QTh"] = B.last("pe")
        finish(4 + j)

    woff = {1: 0, 4: 1024, 16: 1024 + 1408}
    for hs in range(4):
        first = [True, True]
        for g, r in enumerate(DILS):
            h = 4 * g + hs
            evq = load_q(h)
            bi, evkv = load_kv(h, 5120, 3072)
            for qt in range(2):
                qb = 512 * qt
                kbs = [kb for kb in range(qb - 128 * r, qb + 512, 128) if kb >= -2048]
                for ki, kb in enumerate(kbs):
                    dl = qb - kb
                    ti = (kb + 2048) // 128
                    w0 = woff[r] + dl + 384
                    extras = [(ident[:, :], wtab[:, w0:w0 + 512]), (sl6[:, h * 128:(h + 1) * 128], q6[:, :])]
                    didx = h * 40 + qt * 20 + (dl + 384) // 128
                    last = (g == 2 and ki == len(kbs) - 1)
                    pair(kbuf[bi][:, ti * 128:(ti + 1) * 128], QTh[:, qb:qb + 512], extras, db[:, didx:didx + 1],
                         vbuf[bi][:, ti, :], qt, first[qt], last, (evq, evkv))
                    first[qt] = False
            flush()
            B.free[("kv", bi)] = B.last("pe")
            B.free["QTh"] = B.last("pe")
        finish(hs)

    barrier()
    if debug:
        B.dma("sp", dbg_y, yT[:].rearrange("p k t -> p (k t)"), s_st)
    ring_state["rel"] = {}
    ev_y = B.last("dve")
    for tt in range(2):
        tsl = slice(tt * 512, tt * 512 + 512)
        stages = []
        for n in range(16):
            def comp(h, n=n, tt=tt, tsl=tsl):
                t = h["t"]
                evA = mm(("m", 0), ps[0][:, :], [(t[:, kc * 128:(kc + 1) * 128], yT[:, kc, tsl]) for kc in range(4)], extra_wait=(ev_y,))
                evB = mm(("m", 1), ps[1][:, :], [(t[:, 512 + kc * 128:512 + (kc + 1) * 128], yT[:, 4 + kc, tsl]) for kc in range(8)])
                evGa = mm(("m", 2), ps[2][:, :], [(t[:, 1536 + kc * 128:1536 + (kc + 1) * 128], uT[:, kc, tsl]) for kc in range(16)])
                evGb = mm(("m", 3), ps[3][:, :], [(t[:, 3584 + kc * 128:3584 + (kc + 1) * 128], uT[:, kc, tsl]) for kc in range(16)])
                B.wait("act", evGa)
                B.wait("act", B.free.get(("sg", 0)))
                e1 = B.op("act", lambda e: e.activation(out=sg[0][:], in_=ps[2][:, :], func=AF.Sigmoid), sig=True)
                B.free[("m", 2)] = e1
                B.wait("act", evGb)
                B.wait("act", B.free.get(("sg", 1)))
                e3 = B.op("act", lambda e: e.activation(out=sg[1][:], in_=ps[3][:, :], func=AF.Sigmoid), sig=True)
                B.free[("m", 3)] = e3
                B.wait("dve", e1)
                B.wait("dve", evA)
                B.wait("dve", B.free.get(("tmp", 0)))
                e2 = B.op("dve", lambda e: e.tensor_tensor(out=tmp[0][:], in0=sg[0][:], in1=ps[0][:, :], op=ALU.mult), sig=True)
                B.free[("sg", 0)] = e2
                B.free[("m", 0)] = e2
                B.wait("dve", e3)
                B.wait("dve", evB)
                B.wait("dve", B.free.get(("tmp", 1)))
                e4 = B.op("dve", lambda e: e.tensor_tensor(out=tmp[1][:], in0=sg[1][:], in1=ps[1][:, :], op=ALU.mult), sig=True)
                B.free[("sg", 1)] = e4
                B.free[("m", 1)] = e4
                B.wait("dve", e4)
                B.wait("dve", e2)
                B.wait("dve", B.free.get("merged"))
                e5 = B.op("dve", lambda e, n=n: e.tensor_tensor(out=merged[:, n, :], in0=tmp[0][:], in1=tmp[1][:], op=ALU.add), sig=True)
                B.free[("tmp", 0)] = e5
                B.free[("tmp", 1)] = e5
            stages.append(([(lambda t: t[:, 0:5632], wmg_d[n])], comp))
        for n in range(16):
            def comp(h, n=n, tt=tt, tsl=tsl):
                t = h["t"]
                par = 4 + n % 2
                evo = mm(("m", par), ps[par][:, :], [(t[:, kc * 128:(kc + 1) * 128], merged[:, kc, :]) for kc in range(16)],
                         extra_wait=(B.last("dve"),))
                B.wait("dve", evo)
                evx = B.op("dve", lambda e, n=n, par=par, tsl=tsl: e.scalar_tensor_tensor(
                    out=xT[:, n, tsl], in0=ps[par][:, :], scalar=ada[:, 80 + n:80 + n + 1], in1=xT[:, n, tsl],
                    op0=ALU.mult, op1=ALU.add), sig=True)
                B.free[("m", par)] = evx
                if n == 15:
                    B.free["merged"] = B.last("pe")
            stages.append(([(lambda t: t[:, 0:2048], wo_d[n])], comp))
        run_stages(stages)

    if debug:
        B.wait("sp", B.last("dve"))
        ev_dbg = B.dma("sp", dbg_x2, xT[:].rearrange("p k t -> p (k t)"), s_st)
        B.wait("dve", ev_dbg)
    B.free[("A", 0)] = B.last("dve"); B.free[("A", 1)] = B.last("dve")
    B.free[("Bk", 0)] = B.last("dve"); B.free[("Bk", 1)] = B.last("dve")
    B.free[("Cc", 0)] = B.last("act"); B.free[("Cc", 1)] = B.last("act")
    ev_x2 = B.last("dve")
    B.wait("act", ev_x2)
    ev_u3 = norm_mod(32, 96, ev_x2)
    run_stages(ffn(1, 64, ev_u3))
    B.wait("sp", B.last("dve"))
    evo = B.dma("sp", out_d, xT[:].rearrange("p k t -> p (k t)"), s_out)
    B.wait("sp", evo)

    with nc.Block() as block:
        @block.tensor
        def _(e):
            for fn in B.q["pe"]:
                fn(e)

        @block.scalar
        def _(e):
            for fn in B.q["act"]:
                fn(e)

        @block.vector
        def _(e):
            for fn in B.q["dve"]:
                fn(e)

        @block.gpsimd
        def _(e):
            for fn in B.q["pool"]:
                fn(e)

        @block.sync
        def _(e):
            for fn in B.q["sp"]:
                fn(e)
    return nc


def _bf(a):
    return np.ascontiguousarray(a.astype(ml_dtypes.bfloat16))


def _split3(v):
    v = v.astype(np.float32)
    hi = v.astype(ml_dtypes.bfloat16).astype(np.float32)
    mid = (v - hi).astype(ml_dtypes.bfloat16).astype(np.float32)
    lo = (v - hi - mid).astype(ml_dtypes.bfloat16).astype(np.float32)
    return hi, mid, lo


def _kmajor(w, ncols_blk):
    K, N = w.shape
    nb = N // ncols_blk
    return np.ascontiguousarray(w.reshape(K // 128, 128, nb, ncols_blk).transpose(2, 1, 0, 3).reshape(nb, 128, (K // 128) * ncols_blk))


def _vec(v):
    return np.ascontiguousarray(v.reshape(-1, 128).T)


def _const_tables():
    sl = _slopes()
    t = {}
    es = np.zeros((32, 32, 128), np.float32)
    for n in range(32):
        es[n, n, :] = 1.0
    t["esel"] = _bf(es.reshape(32, 32 * 128))
    k = np.arange(128)[:, None]
    q = np.arange(512)[None, :]
    om = np.zeros((128, 4, 512), np.float32)
    for jj in range(4):
        tk = 128 * jj + k
        bad = ((q // 256) == (tk // 256)) & (q < tk)
        om[:, jj, :] = np.where(bad, NEG, 0.0)
    t["ownmask"] = _bf(om.reshape(128, 2048))
    ws = []
    for r in DILS:
        width = 128 * r + 384 + 512
        col = np.arange(width)[None, :]
        x = col - 384 - k
        ok = (x >= 0) & (x <= 128 * r) & (x % r == 0)
        ws.append(np.where(ok, 0.0, NEG).astype(np.float32))
    wt = np.concatenate(ws, axis=1)
    assert wt.shape[1] == 5376, wt.shape
    t["wtab"] = _bf(wt)
    s6 = np.zeros((6, 12, 128), np.float32)
    for h in range(12):
        hi, mid, lo = _split3(np.array([sl[h]], np.float32))
        for i, v in enumerate((hi, hi, mid, mid, lo, lo)):
            s6[i, h, :] = -v[0]
    t["sl6"] = _bf(s6.reshape(6, 12 * 128))
    qo = np.arange(512, dtype=np.float32)
    qhi = qo.astype(ml_dtypes.bfloat16).astype(np.float32)
    qlo = qo - qhi
    t["q6"] = _bf(np.stack([qhi, qlo, qhi, qlo, qhi, qlo], 0))
    t["ident"] = _bf(np.eye(128, dtype=np.float32))
    return t


def _core_tables(c):
    sl = _slopes()
    t = {}
    p = np.arange(128)[:, None]
    pm = np.zeros((128, 8, 32), np.float32)
    past = np.zeros((128, 8, 32), np.float32)
    for stq in range(8):
        qblk = 4 * c + (stq * 128 + p) // 256
        ntrue = (np.arange(32)[None, :] + 4 * (c + 1)) % 32
        ok = ntrue < qblk
        past[:, stq, :] = ok
        pm[:, stq, :] = np.where(ok, 0.0, -1e30)
    own = np.zeros((128, 8, 32), np.float32)
    for stq in range(8):
        qblk = 4 * c + (stq * 128 + p) // 256
        ntrue = (np.arange(32)[None, :] + 4 * (c + 1)) % 32
        own[:, stq, :] = (ntrue == qblk)
    t["own01"] = own.reshape(128, 256)
    t["pm"] = pm.reshape(128, 256)
    t["past01"] = past.reshape(128, 256)
    mb = np.zeros((128, 8, 64, 2), np.float32)
    for j in range(8):
        for kt in range(64):
            trot = kt * 128 + np.arange(128)
            ttrue = (trot + 1024 * (c + 1)) % T
            for qt in range(2):
                tref = 1024 * c + 512 * qt + 511
                v = sl[12 + j] * (ttrue - tref).astype(np.float32)
                mb[:, j, kt, qt] = np.where(ttrue <= tref, v, 0.0)
    t["mbias"] = mb.reshape(128, 8 * 128)
    db = np.zeros((128, 12, 2, 20), np.float32)
    koff = np.arange(128)
    for h in range(12):
        r = DILS[h // 4]
        for qt in range(2):
            for di in range(20):
                dl = di * 128 - 384
                kb = 512 * qt - dl
                v = sl[h] * (koff - dl).astype(np.float32)
                ttrue = 1024 * c + kb + koff
                v = np.where(ttrue >= 0, v, NEG)
                db[:, h, qt, di] = v
    dbf = np.zeros((128, 512), np.float32)
    dbf[:, :480] = db.reshape(128, 480)
    t["dbias"] = dbf
    return t


_CACHE = {}


def kernel(x, c, w_ada, b_ada, g_ffn1, ffn1_w_gate, ffn1_w_up, ffn1_w_down, g_mix, w_in, q_norm, k_norm, w_gate,
           w_branch_a, w_branch_b, w_out, g_ffn2, ffn2_w_gate, ffn2_w_up, ffn2_w_down):
    f = np.float32
    x = np.asarray(x, f)[0]
    shared = {}
    shared["csb"] = _vec(np.asarray(c, f)[0])
    wa = np.asarray(w_ada, f)[0]
    shared["wada"] = _kmajor(wa, 384)
    shared["bada"] = _vec(np.asarray(b_ada, f)[0])
    shared["gains"] = np.ascontiguousarray(np.concatenate([_vec(np.asarray(g, f)[0]) for g in (g_ffn1, g_mix, g_ffn2)], 1))
    shared["qkg"] = np.ascontiguousarray(np.concatenate([np.asarray(q_norm, f)[0].T, np.asarray(k_norm, f)[0].T], 1))
    for nm, (wg, wu, wd) in (("f1", (ffn1_w_gate, ffn1_w_up, ffn1_w_down)), ("f2", (ffn2_w_gate, ffn2_w_up, ffn2_w_down))):
        g = _kmajor(np.asarray(wg, f)[0], 128)
        u = _kmajor(np.asarray(wu, f)[0], 128)
        shared[nm + "_wgu"] = np.ascontiguousarray(np.concatenate([g, u], axis=2))
        shared[nm + "_wd"] = np.ascontiguousarray(np.asarray(wd, f)[0].reshape(NF, 128, 2048))
    wi = np.asarray(w_in, f)[0]
    qcols = np.concatenate([wi[:, 0:1536], wi[:, 4608:5632]], 1)
    kcols = np.concatenate([wi[:, 1536:3072], wi[:, 5632:6656]], 1)
    vcols = np.concatenate([wi[:, 3072:4608], wi[:, 6656:7680]], 1)
    shared["wq"] = _kmajor(qcols, 128)
    shared["wk"] = _kmajor(kcols, 128)
    shared["wv"] = _kmajor(vcols, 256)
    wgt = np.asarray(w_gate, f)[0]
    wba = np.asarray(w_branch_a, f)[0]
    wbb = np.asarray(w_branch_b, f)[0]
    wmg = np.zeros((16, 128, 5632), f)
    wmg[:, :, 0:512] = wba.reshape(4, 128, 16, 128).transpose(2, 1, 0, 3).reshape(16, 128, 512)
    wmg[:, :, 512:1536] = wbb.reshape(8, 128, 16, 128).transpose(2, 1, 0, 3).reshape(16, 128, 1024)
    wmg[:, :, 1536:3584] = _kmajor(wgt[:, 0:2048], 128)
    wmg[:, :, 3584:5632] = _kmajor(wgt[:, 2048:4096], 128)
    shared["wmg"] = wmg
    shared["wo"] = _kmajor(np.asarray(w_out, f)[0], 128)
    shared.update(_const_tables())
    in_maps = []
    for ci in range(NC):
        m = dict(shared)
        xr = np.roll(x, -1024 * (ci + 1), axis=0)
        m["xT"] = np.ascontiguousarray(xr.reshape(8, 1024, 16, 128).transpose(0, 3, 2, 1).reshape(8, 128, 16 * 1024))
        m.update(_core_tables(ci))
        in_maps.append(m)
    if _CACHE.get("debug_hook") is not None:
        return _CACHE["debug_hook"](in_maps)
    if "nc" not in _CACHE:
        _CACHE["nc"] = _build()
    res = run_bass_kernel_spmd(_CACHE["nc"], in_maps, core_ids=list(range(NC)))
    out = np.empty((T, D), f)
    for ci in range(NC):
        o = np.asarray(res.results[ci]["outT"]).reshape(128, 16, 1024)
        out[1024 * ci:1024 * (ci + 1)] = o.transpose(2, 1, 0).reshape(1024, 2048)
    return out[None]
```
